# Optimizing a Trainium2 kernel written in Bass

```python
import math
import jax
import jax.numpy as jnp
from jax import lax
import numpy as np


D_MODEL = 2048
BATCH = 1
SEQ = 8192
DEPTH = 4

CHUNK = 64
QBLK = 128
EPS = 1e-6
N_MIXERS = 3
ROPE_BASE = 10000.0
PLE_DIM = 256
FFN_HIDDEN = ((8 * D_MODEL // 3 + 255) // 256) * 256

A_HEADS = 16
A_KV_HEADS = 4
A_GROUP = A_HEADS // A_KV_HEADS
A_HEAD_DIM = 128
IDX_HEADS = 16
IDX_DIM = 128
IDX_ROPE = 64
IDX_TOPK_MAX = 256
A_Q = A_HEADS * A_HEAD_DIM
A_KV = A_KV_HEADS * A_HEAD_DIM
A_IDXQ = IDX_HEADS * IDX_DIM
A_SPLITS = (A_Q, A_Q + A_KV, A_Q + 2 * A_KV, A_Q + 2 * A_KV + A_IDXQ, A_Q + 2 * A_KV + A_IDXQ + IDX_DIM)
A_IN = A_SPLITS[-1] + IDX_HEADS

T5_BUCKETS = 32
T5_MAX_DISTANCE = 1024

B_HEADS = 32
B_HEAD_DIM = 64
B_PREV_CHUNKS = 8
B_BAND = (B_PREV_CHUNKS + 1) * CHUNK
B_PAD = B_PREV_CHUNKS * CHUNK
B_REL_CLIP = 128

C_HEADS = 16
C_Q_LORA = 512
C_KV_LORA = 512
C_NOPE = 128
C_ROPE = 64
C_V = 128
C_DOWN = C_Q_LORA + C_KV_LORA + C_ROPE

N_A = (DEPTH + 2) // 3
N_B = (DEPTH + 1) // 3
N_C = DEPTH // 3

kernel_name = "hybrid_dsa_chunkband_mla_trunk"


def rms_norm(x, g):
    xf = x.astype(jnp.float32)
    y = xf * lax.rsqrt(jnp.mean(xf * xf, axis=-1, keepdims=True) + EPS)
    return (y * g.astype(jnp.float32)).astype(x.dtype)


def rope(x, pos):
    half = x.shape[-1] // 2
    inv = ROPE_BASE ** (-jnp.arange(half, dtype=jnp.float32) * 2.0 / x.shape[-1])
    ang = pos.astype(jnp.float32)[..., None] * inv
    ang = ang.reshape(ang.shape[:2] + (1,) * (x.ndim - 3) + (half,))
    cos, sin = jnp.cos(ang), jnp.sin(ang)
    xf = x.astype(jnp.float32)
    x1, x2 = xf[..., :half], xf[..., half:]
    return jnp.concatenate([x1 * cos - x2 * sin, x1 * sin + x2 * cos], axis=-1).astype(x.dtype)


def t5_bucket(rel):
    nb = T5_BUCKETS // 2
    max_exact = nb // 2
    ret = jnp.where(rel > 0, nb, 0)
    n = jnp.abs(rel)
    nf = jnp.maximum(n, 1).astype(jnp.float32)
    large = max_exact + (jnp.log(nf / max_exact) / math.log(T5_MAX_DISTANCE / max_exact) * (nb - max_exact)).astype(jnp.int32)
    large = jnp.minimum(large, nb - 1)
    return ret + jnp.where(n < max_exact, n, large)


def to_blocks(a, size):
    b, s = a.shape[:2]
    return jnp.moveaxis(a.reshape((b, s // size, size) + a.shape[2:]), 1, 0)


def from_blocks(o):
    o = jnp.moveaxis(o, 0, 1)
    return o.reshape(o.shape[0], o.shape[1] * o.shape[2], o.shape[3])


def swiglu(h, w_in, w_out):
    gu = h @ w_in
    g, u = gu[..., :FFN_HIDDEN], gu[..., FFN_HIDDEN:]
    return (jax.nn.silu(g) * u) @ w_out


def dsa_sparse_attention(h, pos, w_in, w_out, t5_table):
    b, s, _ = h.shape
    topk = min(IDX_TOPK_MAX, s // 4)
    nb = s // QBLK
    q, k, v, qi, ki, wi = jnp.split(h @ w_in, A_SPLITS, axis=-1)
    q = q.reshape(b, s, A_KV_HEADS, A_GROUP, A_HEAD_DIM)
    k = k.reshape(b, s, A_KV_HEADS, A_HEAD_DIM)
    v = v.reshape(b, s, A_KV_HEADS, A_HEAD_DIM)
    qi = qi.reshape(b, s, IDX_HEADS, IDX_DIM)
    qi = jnp.concatenate([rope(qi[..., :IDX_ROPE], pos), qi[..., IDX_ROPE:]], axis=-1)
    ki = jnp.concatenate([rope(ki[..., :IDX_ROPE], pos), ki[..., IDX_ROPE:]], axis=-1)
    wi = wi * IDX_HEADS ** -0.5
    cid = jnp.arange(s) // CHUNK
    take = jax.vmap(lambda a, i: a[i])

    def block(args):
        qb, qib, wib, pb, bi = args
        qc = (bi * QBLK + jnp.arange(QBLK)) // CHUNK
        sc = jnp.einsum('bqhd,bsd->bqhs', qib, ki).astype(jnp.float32) * IDX_DIM ** -0.5
        score = jnp.einsum('bqhs,bqh->bqs', jax.nn.relu(sc), wib.astype(jnp.float32))
        score = jnp.where(cid[None, None, :] <= qc[None, :, None], score, -jnp.inf)
        _, idx = lax.top_k(score, topk)
        valid = cid[idx] <= qc[None, :, None]
        kg = take(k, idx)
        vg = take(v, idx)
        pg = take(pos, idx)
        bias = t5_table[t5_bucket(pg - pb[:, :, None])]
        bias = jnp.moveaxis(bias.reshape(b, QBLK, topk, A_KV_HEADS, A_GROUP), 2, -1)
        logits = jnp.einsum('bqkgd,bqskd->bqkgs', qb, kg).astype(jnp.float32) * A_HEAD_DIM ** -0.5 + bias.astype(jnp.float32)
        logits = jnp.where(valid[:, :, None, None, :], logits, -jnp.inf)
        pr = jax.nn.softmax(logits, axis=-1).astype(vg.dtype)
        o = jnp.einsum('bqkgs,bqskd->bqkgd', pr, vg)
        return o.reshape(b, QBLK, A_HEADS * A_HEAD_DIM)

    o = lax.map(block, (to_blocks(q, QBLK), to_blocks(qi, QBLK), to_blocks(wi, QBLK), to_blocks(pos, QBLK), jnp.arange(nb)))
    return from_blocks(o) @ w_out


def chunk_band_attention(h, pos, w_in, rel_table, w_out):
    b, s, _ = h.shape
    nc = s // CHUNK
    q, k, v = jnp.split(h @ w_in, 3, axis=-1)
    q = q.reshape(b, s, B_HEADS, B_HEAD_DIM)
    k = jnp.pad(k.reshape(b, s, B_HEADS, B_HEAD_DIM), ((0, 0), (B_PAD, 0), (0, 0), (0, 0)))
    v = jnp.pad(v.reshape(b, s, B_HEADS, B_HEAD_DIM), ((0, 0), (B_PAD, 0), (0, 0), (0, 0)))
    pp = jnp.pad(pos, ((0, 0), (B_PAD, 0)))

    def per_chunk(c):
        s0 = c * CHUNK
        qc = lax.dynamic_slice_in_dim(q, s0, CHUNK, axis=1)
        pq = lax.dynamic_slice_in_dim(pos, s0, CHUNK, axis=1)
        kb = lax.dynamic_slice_in_dim(k, s0, B_BAND, axis=1)
        vb = lax.dynamic_slice_in_dim(v, s0, B_BAND, axis=1)
        pb = lax.dynamic_slice_in_dim(pp, s0, B_BAND, axis=1)
        valid = (s0 - B_PAD + jnp.arange(B_BAND)) >= 0
        rel = jnp.clip(pq[:, :, None] - pb[:, None, :], -B_REL_CLIP, B_REL_CLIP) + B_REL_CLIP
        bias = jnp.moveaxis(rel_table[rel], -1, 1)
        logits = jnp.einsum('bqhd,bkhd->bhqk', qc, kb).astype(jnp.float32) * B_HEAD_DIM ** -0.5 + bias.astype(jnp.float32)
        logits = jnp.where(valid, logits, -jnp.inf)
        pr = jax.nn.softmax(logits, axis=-1).astype(vb.dtype)
        o = jnp.einsum('bhqk,bkhd->bqhd', pr, vb)
        return o.reshape(b, CHUNK, B_HEADS * B_HEAD_DIM)

    o = lax.map(per_chunk, jnp.arange(nc))
    return from_blocks(o) @ w_out


def mla_attention(h, pos, w_down, g_q, g_kv, w_uq, w_ukv, w_out):
    b, s, _ = h.shape
    nb = s // QBLK
    cq, ckv, kr = jnp.split(h @ w_down, (C_Q_LORA, C_Q_LORA + C_KV_LORA), axis=-1)
    cq = rms_norm(cq, g_q)
    ckv = rms_norm(ckv, g_kv)
    q = (cq @ w_uq).reshape(b, s, C_HEADS, C_NOPE + C_ROPE)
    qn, qr = q[..., :C_NOPE], rope(q[..., C_NOPE:], pos)
    kv = (ckv @ w_ukv).reshape(b, s, C_HEADS, C_NOPE + C_V)
    kn, v = kv[..., :C_NOPE], kv[..., C_NOPE:]
    kr = rope(kr, pos)
    cid = jnp.arange(s) // CHUNK
    scale = (C_NOPE + C_ROPE) ** -0.5

    def block(args):
        qnb, qrb, bi = args
        qc = (bi * QBLK + jnp.arange(QBLK)) // CHUNK
        logits = (jnp.einsum('bqhd,bkhd->bhqk', qnb, kn) + jnp.einsum('bqhd,bkd->bhqk', qrb, kr)).astype(jnp.float32) * scale
        logits = jnp.where(cid[None, :] <= qc[:, None], logits, -jnp.inf)
        pr = jax.nn.softmax(logits, axis=-1).astype(v.dtype)
        o = jnp.einsum('bhqk,bkhd->bqhd', pr, v)
        return o.reshape(b, QBLK, C_HEADS * C_V)

    o = lax.map(block, (to_blocks(qn, QBLK), to_blocks(qr, QBLK), jnp.arange(nb)))
    return from_blocks(o) @ w_out


def setup_inputs(seed: int = 0) -> dict:
    key = jax.random.key(seed)
    ks = jax.random.split(key, 24)
    f32 = jnp.float32

    def dense(k, shape):
        return jax.random.normal(k, shape, f32) * shape[-2] ** -0.5

    def gain(k, shape):
        return 1.0 + 0.05 * jax.random.normal(k, shape, f32)

    x = jax.random.normal(ks[0], (BATCH, SEQ, D_MODEL), f32)
    p = jax.random.normal(ks[1], (DEPTH, BATCH, SEQ, PLE_DIM), f32)
    offset = jax.random.randint(ks[2], (BATCH, 1), 0, 1024, jnp.int32)
    positions = offset + jnp.arange(SEQ, dtype=jnp.int32)[None, :]
    return {
        "x": x,
        "p": p,
        "positions": positions,
        "t5_table": 0.5 * jax.random.normal(ks[3], (T5_BUCKETS, A_HEADS), f32),
        "a_w_in": dense(ks[4], (N_A, D_MODEL, A_IN)),
        "a_w_out": dense(ks[5], (N_A, A_HEADS * A_HEAD_DIM, D_MODEL)),
        "b_w_in": dense(ks[6], (N_B, D_MODEL, 3 * B_HEADS * B_HEAD_DIM)),
        "b_rel_table": 0.5 * jax.random.normal(ks[7], (N_B, 2 * B_REL_CLIP + 1, B_HEADS), f32),
        "b_w_out": dense(ks[8], (N_B, B_HEADS * B_HEAD_DIM, D_MODEL)),
        "c_w_down": dense(ks[9], (N_C, D_MODEL, C_DOWN)),
        "c_q_norm": gain(ks[10], (N_C, C_Q_LORA)),
        "c_kv_norm": gain(ks[11], (N_C, C_KV_LORA)),
        "c_w_uq": dense(ks[12], (N_C, C_Q_LORA, C_HEADS * (C_NOPE + C_ROPE))),
        "c_w_ukv": dense(ks[13], (N_C, C_KV_LORA, C_HEADS * (C_NOPE + C_V))),
        "c_w_out": dense(ks[14], (N_C, C_HEADS * C_V, D_MODEL)),
        "attn_norm": gain(ks[15], (DEPTH, D_MODEL)),
        "ffn_norm": gain(ks[16], (DEPTH, D_MODEL)),
        "ffn_w_in": dense(ks[17], (DEPTH, D_MODEL, 2 * FFN_HIDDEN)),
        "ffn_w_out": dense(ks[18], (DEPTH, FFN_HIDDEN, D_MODEL)),
        "ple_norm": gain(ks[19], (DEPTH, D_MODEL)),
        "ple_w_gate": dense(ks[20], (DEPTH, D_MODEL, D_MODEL)),
        "ple_w_proj": dense(ks[21], (DEPTH, PLE_DIM, D_MODEL)),
        "final_norm": gain(ks[22], (D_MODEL,)),
    }


def reference(x, p, positions, t5_table, a_w_in, a_w_out, b_w_in, b_rel_table, b_w_out,
              c_w_down, c_q_norm, c_kv_norm, c_w_uq, c_w_ukv, c_w_out,
              attn_norm, ffn_norm, ffn_w_in, ffn_w_out, ple_norm, ple_w_gate, ple_w_proj, final_norm):
    h = x
    for i in range(DEPTH):
        j = i // N_MIXERS
        kind = i % N_MIXERS
        hn = rms_norm(h, attn_norm[i])
        if kind == 0:
            mix = dsa_sparse_attention(hn, positions, a_w_in[j], a_w_out[j], t5_table)
        elif kind == 1:
            mix = chunk_band_attention(hn, positions, b_w_in[j], b_rel_table[j], b_w_out[j])
        else:
            mix = mla_attention(hn, positions, c_w_down[j], c_q_norm[j], c_kv_norm[j], c_w_uq[j], c_w_ukv[j], c_w_out[j])
        h = h + mix
        h = h + swiglu(rms_norm(h, ffn_norm[i]), ffn_w_in[i], ffn_w_out[i])
        gate = jax.nn.sigmoid(rms_norm(h, ple_norm[i]) @ ple_w_gate[i])
        h = h + gate * (p[i] @ ple_w_proj[i])
    return rms_norm(h, final_norm)
```

```python
import ml_dtypes
import numpy as np
from contextlib import ExitStack
import concourse.bass as bass
import concourse.mybir as mybir
from concourse.bass_utils import run_bass_kernel_spmd

F32 = mybir.dt.float32
BF16 = mybir.dt.bfloat16
I32 = mybir.dt.int32
AF = mybir.ActivationFunctionType
ALU = mybir.AluOpType
AX = mybir.AxisListType
DT_SIZE = {F32: 4, BF16: 2, I32: 4}

COMPUTE = ("pe", "act", "dve", "pool")
EPOCH = 30000
NDMASEM = 20


def region(ap):
    t = ap.tensor
    name = t.name
    pairs = list(ap.ap)
    off = int(ap.offset)
    esz = DT_SIZE.get(ap.dtype, 4)
    sp = str(ap.space).lower() if hasattr(ap, "space") else ""
    if "dram" in sp or "hbm" in sp:
        ext = sum((int(c) - 1) * abs(int(s)) for s, c in pairs)
        return (name, 0, 1, off * esz, (off + ext + 1) * esz)
    pstep, pcnt = int(pairs[0][0]), int(pairs[0][1])
    if pstep == 0:
        pstep = 1 << 40
    p0 = off // pstep if pstep < (1 << 39) else 0
    lo = off - p0 * pstep if pstep < (1 << 39) else off
    ext = sum((int(c) - 1) * abs(int(s)) for s, c in pairs[1:])
    return (name, p0, p0 + pcnt, lo * esz, (lo + ext + 1) * esz)


class Op:
    __slots__ = ("eng", "fn", "idx", "deps_c", "deps_d", "signal", "ticket", "dma", "dsem", "dticket", "prev_on_sem")

    def __init__(self, eng, fn, dma):
        self.eng = eng
        self.fn = fn
        self.dma = dma
        self.deps_c = {}
        self.deps_d = []
        self.signal = False
        self.ticket = None


class Sched:
    def __init__(self, nc):
        self.nc = nc
        self.ops = {e: [] for e in COMPUTE + ("sp",)}
        self.mem = {}
        self.nops = 0

    def _overlap(self, e, r):
        return e[0] < r[2] and r[1] < e[1] and e[2] < r[4] and r[3] < e[3]

    def _dep(self, op, src):
        if src is None or src is op:
            return
        if src.dma:
            if src not in op.deps_d:
                op.deps_d.append(src)
        else:
            if src.eng == op.eng and op.eng == "pe" and not op.dma:
                return
            cur = op.deps_c.get(src.eng)
            if cur is None or cur.idx < src.idx:
                op.deps_c[src.eng] = src

    def add(self, eng, fn, reads=(), writes=(), dma=False):
        op = Op(eng, fn, dma)
        lst = self.ops[eng]
        op.idx = len(lst)
        lst.append(op)
        self.nops += 1
        reads = list(reads)
        writes = list(writes)
        pr = [ap for ap in reads if not isinstance(ap, tuple) and ap.tensor.name.startswith("psum")]
        if pr:
            reads = [ap for ap in reads if isinstance(ap, tuple) or not ap.tensor.name.startswith("psum")]
            writes = writes + pr
        for ap in reads:
            r = region(ap) if not isinstance(ap, tuple) else ap
            ents = self.mem.setdefault(r[0], [])
            for e in ents:
                if self._overlap(e, r):
                    self._dep(op, e[4])
                    e[5][("d%d" % id(op)) if dma else eng] = op
        for ap in writes:
            r = region(ap) if not isinstance(ap, tuple) else ap
            if r[0].startswith("psum"):
                r = (r[0], 0, 128, 0, 1 << 30)
            ents = self.mem.setdefault(r[0], [])
            keep = []
            for e in ents:
                if self._overlap(e, r):
                    self._dep(op, e[4])
                    for rd in e[5].values():
                        self._dep(op, rd)
                    if r[1] <= e[0] and e[1] <= r[2] and r[3] <= e[2] and e[3] <= r[4]:
                        continue
                keep.append(e)
            keep.append([r[1], r[2], r[3], r[4], op, {}])
            self.mem[r[0]] = keep
        return op

    def pe(self, fn, reads, writes):
        return self.add("pe", fn, reads, writes)

    def act(self, fn, reads, writes):
        return self.add("act", fn, reads, writes)

    def dve(self, fn, reads, writes):
        return self.add("dve", fn, reads, writes)

    def pool(self, fn, reads, writes):
        return self.add("pool", fn, reads, writes)

    def dma(self, q, out, in_, **kw):
        return self.add(q, lambda e: e.dma_start(out=out, in_=in_, **kw), [in_], [out], dma=True)

    def fence(self, eng, regions):
        return self.add(eng, None, regions, [])

    def emit(self, stack):
        nc = self.nc
        for eng, lst in self.ops.items():
            for op in lst:
                for s in op.deps_c.values():
                    s.signal = True
        csem = {}
        for eng in COMPUTE:
            n = 0
            for op in self.ops[eng]:
                if op.dma:
                    continue
                if op.signal:
                    n += 1
                    op.ticket = n
            nsem = (n + EPOCH - 1) // EPOCH
            csem[eng] = [stack.enter_context(nc.semaphore("c_%s_%d" % (eng, i))) for i in range(max(nsem, 1))]
        dsem = {}
        for eng, lst in self.ops.items():
            nd = sum(1 for op in lst if op.dma)
            if nd == 0:
                continue
            k = min(NDMASEM, nd)
            dsem[eng] = [stack.enter_context(nc.semaphore("d_%s_%d" % (eng, i))) for i in range(k)]
            cnt = [0] * k
            last = [None] * k
            j = 0
            for op in lst:
                if not op.dma:
                    continue
                s = j % k
                j += 1
                cnt[s] += 16
                op.dsem = dsem[eng][s]
                op.dticket = cnt[s]
                op.prev_on_sem = last[s]
                last[s] = op
        block = stack.enter_context(nc.Block())
        stats = {"waits": 0}

        def run_engine(eng, e):
            known_c = {}
            known_d = {}
            for op in self.ops[eng]:
                for src in op.deps_c.values():
                    if known_c.get(src.eng, 0) >= src.ticket:
                        continue
                    known_c[src.eng] = src.ticket
                    k = (src.ticket - 1) // EPOCH
                    e.wait_ge(csem[src.eng][k], (src.ticket - 1) % EPOCH + 1)
                    stats["waits"] += 1
                dd = list(op.deps_d)
                if op.dma and op.prev_on_sem is not None:
                    dd.append(op.prev_on_sem)
                for src in dd:
                    key = id(src.dsem)
                    if known_d.get(key, 0) >= src.dticket:
                        continue
                    known_d[key] = src.dticket
                    e.wait_ge(src.dsem, src.dticket)
                    stats["waits"] += 1
                if op.fn is None:
                    continue
                inst = op.fn(e)
                if op.dma:
                    inst.then_inc(op.dsem, 16)
                elif op.signal:
                    k = (op.ticket - 1) // EPOCH
                    inst.then_inc(csem[eng][k], 1)

        for eng, deco in (("pe", block.tensor), ("act", block.scalar), ("dve", block.vector), ("pool", block.gpsimd), ("sp", block.sync)):
            if self.ops[eng]:
                deco(lambda e, eng=eng: run_engine(eng, e))
        return stats

D = 2048
S = 8192
NCORE = 8
T = 1024
KC = D // 128
EPS = 1e-6
FF = 5632


class M:
    def __init__(self):
        self.nc = bass.Bass("TRN2", target_bir_lowering=False)
        self.stack = ExitStack()
        self.S = Sched(self.nc)
        self.din = {}
        self.dout = {}

    def inp(self, name, shape, dt=F32):
        ap = self.nc.dram_tensor(name, list(shape), dt, kind="ExternalInput").ap()
        self.din[name] = ap
        return ap

    def outp(self, name, shape, dt=F32):
        ap = self.nc.dram_tensor(name, list(shape), dt, kind="ExternalOutput").ap()
        self.dout[name] = ap
        return ap

    def scratch(self, name, shape, dt):
        return self.nc.dram_tensor(name, list(shape), dt, kind="Internal").ap()

    def sb(self, name, shape, dt):
        return self.stack.enter_context(self.nc.sbuf_tensor(name, list(shape), dt))

    def ps(self, name, shape, dt=F32):
        return self.stack.enter_context(self.nc.psum_tensor(name, list(shape), dt))

    def finish(self):
        st = self.S.emit(self.stack)
        self.stack.close()
        return st


def setup_common(m):
    S_ = m.S
    m.c_identF = m.inp("c_identF", [128, 128])
    m.identF = m.sb("identF", [128, 128], F32)
    m.identB = m.sb("identB", [128, 128], BF16)
    m.onesB = m.sb("onesB", [128, 128], BF16)
    m.hT = m.sb("hT", [128, KC, T], F32)
    m.hnT = m.sb("hnT", [128, KC, T], BF16)
    m.psum = [m.ps("psum%d" % i, [128, 512], F32) for i in range(6)]
    m.psi = 0
    m.rstd = m.sb("rstd", [128, T], F32)
    m.psumT = [m.ps("psumT%d" % i, [128, 1024], BF16) for i in range(2)]
    S_.dma("sp", m.identF[:], m.c_identF[:, :])
    S_.dve(lambda e: e.tensor_copy(out=m.identB[:], in_=m.identF[:]), [m.identF[:]], [m.identB[:]])
    S_.dve(lambda e: e.memset(m.onesB[:], 1.0 / D), [], [m.onesB[:]])
    m.epsc = m.sb("epsc", [128, 1], F32)
    S_.dve(lambda e: e.memset(m.epsc[:], EPS), [], [m.epsc[:]])


def next_psum(m):
    p = m.psum[m.psi % 6]
    m.psi += 1
    return p


def load_x(m, x_dram, stage_buf):
    S_ = m.S
    for j in range(T // 128):
        xt = stage_buf[j % 2]
        S_.dma("sp", xt[:], x_dram[j * 128:(j + 1) * 128, :])
        for b in range(KC // 4):
            p = next_psum(m)
            for q in range(4):
                kc = 4 * b + q
                S_.pe(lambda e, p=p, q=q, kc=kc, xt=xt: e.transpose(p[:, q * 128:(q + 1) * 128], xt[:, kc * 128:(kc + 1) * 128], m.identF[:]),
                      [xt[:, kc * 128:(kc + 1) * 128], m.identF[:]], [p[:, q * 128:(q + 1) * 128]])
            dst = m.hT[:, 4 * b:4 * b + 4, j * 128:(j + 1) * 128]
            src = p[:].rearrange("p (a b) -> p a b", a=4)
            eng = S_.act if (b % 2) else S_.dve
            if b % 2:
                S_.act(lambda e, dst=dst, src=src: e.copy(out=dst, in_=src), [p[:]], [dst])
            else:
                S_.dve(lambda e, dst=dst, src=src: e.tensor_copy(out=dst, in_=src), [p[:]], [dst])


def rms_stats(m, rstd, sqbuf):
    S_ = m.S
    for half in range(T // 512):
        sl = slice(half * 512, (half + 1) * 512)
        p = next_psum(m)
        for kc in range(KC):
            sq = sqbuf[kc % 2]
            src = m.hT[:, kc, sl]
            if kc % 2:
                S_.act(lambda e, sq=sq, src=src: e.activation(out=sq[:], in_=src, func=AF.Square), [src], [sq[:]])
            else:
                S_.pool(lambda e, sq=sq, src=src: e.tensor_tensor(out=sq[:], in0=src, in1=src, op=ALU.mult), [src], [sq[:]])
            S_.pe(lambda e, p=p, sq=sq, kc=kc: e.matmul(p[:], m.onesB[:], sq[:], start=(kc == 0), stop=(kc == KC - 1)),
                  [m.onesB[:], sq[:]], [p[:]])
        dst = rstd[:, sl]
        S_.act(lambda e, dst=dst, p=p: e.activation(out=dst, in_=p[:], func=AF.Sqrt, bias=m.epsc[:], scale=1.0), [p[:], m.epsc[:]], [dst])
        S_.dve(lambda e, dst=dst: e.reciprocal(out=dst, in_=dst), [dst], [dst])


def final_norm_out(m, g_sb, rstd, out_dram, ybuf, obuf):
    S_ = m.S
    for j in range(T // 128):
        tsl = slice(j * 128, (j + 1) * 128)
        ob = obuf[j % 2]
        for b in range(KC // 4):
            p = next_psum(m)
            for q in range(4):
                kc = 4 * b + q
                yb = ybuf[kc % 2]
                S_.dve(lambda e, yb=yb, kc=kc, tsl=tsl: e.scalar_tensor_tensor(out=yb[:], in0=m.hT[:, kc, tsl], scalar=g_sb[:, kc:kc + 1], in1=rstd[:, tsl], op0=ALU.mult, op1=ALU.mult),
                       [m.hT[:, kc, tsl], g_sb[:, kc:kc + 1], rstd[:, tsl]], [yb[:]])
                S_.pe(lambda e, p=p, q=q, yb=yb: e.transpose(p[:, q * 128:(q + 1) * 128], yb[:], m.identF[:]),
                      [yb[:], m.identF[:]], [p[:, q * 128:(q + 1) * 128]])
            dst = ob[:, b * 512:(b + 1) * 512]
            S_.act(lambda e, dst=dst, p=p: e.copy(out=dst, in_=p[:]), [p[:]], [dst])
        S_.dma("sp", out_dram[tsl, :], ob[:])


WSLOT = 8192


def setup_linear(m, nslots=3):
    m.wslots = [m.sb("wslot%d" % i, [128, WSLOT], BF16) for i in range(nslots)]
    m.wi = 0
    m.sqb = [m.sb("sq%d" % i, [128, 512], BF16) for i in range(2)]
    m.rstd = m.sb("rstd", [128, T], F32)
    m.tmpf = [m.sb("tmpf%d" % i, [128, 512], F32) for i in range(3)]
    m.tfi = 0


def next_tmp(m):
    t = m.tmpf[m.tfi % len(m.tmpf)]
    m.tfi += 1
    return t


def load_w(m, W, r0, nk, c0, ncols, parts=128):
    slot = m.wslots[m.wi % len(m.wslots)]
    m.wi += 1
    assert nk * ncols <= WSLOT
    v = slot[0:parts, 0:nk * ncols].rearrange("p (k n) -> p k n", k=nk)
    src = W[r0:r0 + nk * parts, c0:c0 + ncols].rearrange("(k p) n -> p k n", p=parts)
    m.S.dma("pool", v, src)
    return v


def rmsnorm_to(m, g_sb, dst, src=None, nkc=KC, ones=None):
    S_ = m.S
    src = m.hT if src is None else src
    ones = m.onesB if ones is None else ones
    for half in range(T // 512):
        sl = slice(half * 512, (half + 1) * 512)
        p = next_psum(m)
        for kc in range(nkc):
            sq = m.sqb[kc % 2]
            s_ = src[:, kc, sl]
            if kc % 2:
                S_.act(lambda e, sq=sq, s_=s_: e.activation(out=sq[:], in_=s_, func=AF.Square), [s_], [sq[:]])
            else:
                S_.pool(lambda e, sq=sq, s_=s_: e.tensor_tensor(out=sq[:], in0=s_, in1=s_, op=ALU.mult), [s_], [sq[:]])
            S_.pe(lambda e, p=p, sq=sq, kc=kc: e.matmul(p[:], ones[:], sq[:], start=(kc == 0), stop=(kc == nkc - 1)),
                  [ones[:], sq[:]], [p[:]])
        r = m.rstd[:, sl]
        S_.act(lambda e, r=r, p=p: e.activation(out=r, in_=p[:], func=AF.Sqrt, bias=m.epsc[:], scale=1.0), [p[:], m.epsc[:]], [r])
        S_.dve(lambda e, r=r: e.reciprocal(out=r, in_=r), [r], [r])
        for kc in range(nkc):
            d_ = dst[:, kc, sl]
            s_ = src[:, kc, sl]
            gg = g_sb[:, kc:kc + 1]
            eng = "dve"
            S_.add(eng, lambda e, d_=d_, s_=s_, gg=gg, r=r: e.scalar_tensor_tensor(out=d_, in0=s_, scalar=gg, in1=r, op0=ALU.mult, op1=ALU.mult),
                   [s_, gg, r], [d_])


def mm_fm(m, p, wv, c_lo, c_n, xT, nk, sl, k_parts=128):
    for k in range(nk):
        lhs = wv[:, k, c_lo:c_lo + c_n]
        rhs = xT[0:k_parts, k, sl]
        m.S.pe(lambda e, lhs=lhs, rhs=rhs, k=k: e.matmul(p[0:c_n, :], lhs, rhs, start=(k == 0), stop=(k == nk - 1)),
               [lhs, rhs], [p[0:c_n, :]])


def ffn(m, w_in, w_out, actT):
    S_ = m.S
    NH = 2
    FH = FF // 128 // NH
    for fh in range(NH):
        for s2 in range(FH // 2):
            f0 = (fh * FH + s2 * 2) * 128
            slot = m.wslots[m.wi % len(m.wslots)]
            m.wi += 1
            wv = slot[:, 0:KC * 512].rearrange("p (k u n) -> p k u n", k=KC, u=2)
            for u in range(2):
                src = w_in[:, u * FF + f0:u * FF + f0 + 256].rearrange("(k p) n -> p k n", p=128)
                S_.dma("pool", wv[:, :, u, :], src)
            for fi in range(2):
                fl = s2 * 2 + fi
                for half in range(2):
                    sl = slice(half * 512, (half + 1) * 512)
                    pg = next_psum(m)
                    pu = next_psum(m)
                    mm_fm(m, pg, wv[:, :, 0, :], fi * 128, 128, m.hnT, KC, sl)
                    mm_fm(m, pu, wv[:, :, 1, :], fi * 128, 128, m.hnT, KC, sl)
                    t = next_tmp(m)
                    S_.act(lambda e, t=t, pg=pg: e.activation(out=t[:], in_=pg[:], func=AF.Silu), [pg[:]], [t[:]])
                    d_ = actT[:, fl, sl]
                    S_.dve(lambda e, d_=d_, t=t, pu=pu: e.tensor_tensor(out=d_, in0=t[:], in1=pu[:], op=ALU.mult), [t[:], pu[:]], [d_])
        for dg in range(KC // 2):
            wv = load_w(m, w_out, fh * FH * 128, FH, dg * 256, 256)
            for di in range(2):
                dc = dg * 2 + di
                for half in range(2):
                    sl = slice(half * 512, (half + 1) * 512)
                    p = next_psum(m)
                    mm_fm(m, p, wv, di * 128, 128, actT, FH, sl)
                    h_ = m.hT[:, dc, sl]
                    S_.dve(lambda e, h_=h_, p=p: e.tensor_tensor(out=h_, in0=h_, in1=p[:], op=ALU.add), [h_, p[:]], [h_])


def load_pT(m, p_dram, pT, stage):
    S_ = m.S
    for j in range(T // 128):
        st = stage[j % 2]
        S_.dma("sp", st[:, 0:256], p_dram[j * 128:(j + 1) * 128, :])
        p = next_psum(m)
        for q in range(2):
            S_.pe(lambda e, p=p, q=q, st=st: e.transpose(p[:, q * 128:(q + 1) * 128], st[:, q * 128:(q + 1) * 128], m.identF[:]),
                  [st[:, q * 128:(q + 1) * 128], m.identF[:]], [p[:, q * 128:(q + 1) * 128]])
        dst = pT[:, :, j * 128:(j + 1) * 128]
        src = p[:, 0:256].rearrange("p (a b) -> p a b", a=2)
        S_.act(lambda e, dst=dst, src=src: e.copy(out=dst, in_=src), [p[:, 0:256]], [dst])


def ple(m, w_gate, w_proj, pT):
    S_ = m.S
    for dg in range(KC // 2):
        slot = m.wslots[m.wi % len(m.wslots)]
        m.wi += 1
        wg = slot[:, 0:KC * 256].rearrange("p (k n) -> p k n", k=KC)
        wp = slot[:, KC * 256:KC * 256 + 512].rearrange("p (k n) -> p k n", k=2)
        S_.dma("pool", wg, w_gate[:, dg * 256:(dg + 1) * 256].rearrange("(k p) n -> p k n", p=128))
        S_.dma("pool", wp, w_proj[:, dg * 256:(dg + 1) * 256].rearrange("(k p) n -> p k n", p=128))
        for di in range(2):
            dc = dg * 2 + di
            for half in range(2):
                sl = slice(half * 512, (half + 1) * 512)
                pg = next_psum(m)
                pp = next_psum(m)
                mm_fm(m, pg, wg, di * 128, 128, m.hnT, KC, sl)
                mm_fm(m, pp, wp, di * 128, 128, pT, 2, sl)
                t = next_tmp(m)
                S_.act(lambda e, t=t, pg=pg: e.activation(out=t[:], in_=pg[:], func=AF.Sigmoid), [pg[:]], [t[:]])
                S_.dve(lambda e, t=t, pp=pp: e.tensor_tensor(out=t[:], in0=t[:], in1=pp[:], op=ALU.mult), [t[:], pp[:]], [t[:]])
                h_ = m.hT[:, dc, sl]
                S_.pool(lambda e, h_=h_, t=t: e.tensor_tensor(out=h_, in0=h_, in1=t[:], op=ALU.add), [h_, t[:]], [h_])


def out_proj(m, w_out, oT):
    S_ = m.S
    for dg in range(KC // 4):
        wv = load_w(m, w_out, 0, KC, dg * 512, 512)
        for di in range(4):
            dc = dg * 4 + di
            for half in range(2):
                sl = slice(half * 512, (half + 1) * 512)
                p = next_psum(m)
                mm_fm(m, p, wv, di * 128, 128, oT, KC, sl)
                h_ = m.hT[:, dc, sl]
                S_.dve(lambda e, h_=h_, p=p: e.tensor_tensor(out=h_, in0=h_, in1=p[:], op=ALU.add), [h_, p[:]], [h_])


def save_h(m, h_dram):
    m.S.dma("sp", h_dram.rearrange("p (k t) -> p k t", k=KC), m.hT[:])


def load_h(m, h_dram):
    m.S.dma("sp", m.hT[:], h_dram.rearrange("p (k t) -> p k t", k=KC))


ARENA_ELEMS = 52224


def setup_arena(m):
    m.arena = m.sb("arena", [128, ARENA_ELEMS], BF16)
    m.aoff = 0


def areset(m, off=0):
    m.aoff = off


def aalloc(m, shape, dt, parts=128):
    n = int(np.prod(shape))
    nb = n * DT_SIZE[dt]
    nb16 = (nb + 3) // 4 * 2
    assert m.aoff + nb16 <= ARENA_ELEMS, ("arena overflow", m.aoff, nb16)
    v = m.arena[0:parts, m.aoff:m.aoff + nb16]
    m.aoff += nb16
    if dt != BF16:
        v = v.bitcast(dt)
    v = v[:, 0:n]
    if len(shape) == 2:
        v = v.rearrange("p (a b) -> p a b", a=shape[0])
    elif len(shape) == 3:
        v = v.rearrange("p (a b c) -> p a b c", a=shape[0], b=shape[1])
    return v


def setup_linear_arena(m):
    areset(m)
    m.wslots = [aalloc(m, [WSLOT], BF16) for _ in range(3)]
    m.wi = 0
    m.tmpf = [aalloc(m, [512], F32) for _ in range(2)]
    m.tfi = 0
    a0 = m.aoff
    m.actT = aalloc(m, [22, T], BF16)
    m.pT = aalloc(m, [2, T], BF16)
    m.pstage = [aalloc(m, [256], F32) for _ in range(2)]
    m.aoff = a0
    m.sqb = [aalloc(m, [512], BF16) for _ in range(2)]
    m.stg = [aalloc(m, [T], BF16) for _ in range(3)]
    m.stgv = [aalloc(m, [512], BF16) for _ in range(3)]
    m.stgf = [aalloc(m, [16], F32) for _ in range(2)]
    m.cT = aalloc(m, [4, T], F32)
    m.cnT = aalloc(m, [4, T], BF16)
    m.xb = [aalloc(m, [512], BF16) for _ in range(2)]
    m.cosT = aalloc(m, [T], F32)
    m.sinT = aalloc(m, [T], F32)
    m.si = 0
    m.svi = 0
    m.xi = 0


def bcast_rows(ap_row, nparts):
    pairs = list(ap_row.ap)
    return bass.AP(ap_row.tensor, ap_row.offset, [[0, nparts]] + [list(x) for x in pairs[1:]])


def setup_consts2(m, invf_dram, rot_dram):
    S_ = m.S
    m.rotB = m.sb("rotB", [128, 128], BF16)
    m.invf = m.sb("invf", [128, 1], F32)
    m.ones512 = m.sb("ones512", [128, 128], BF16)
    S_.dma("sp", m.invf[:], invf_dram[:, :])
    S_.dma("pool", m.rotB[:], rot_dram[:, :])
    S_.dve(lambda e: e.memset(m.ones512[:], 1.0 / 512), [], [m.ones512[:]])


def setup_rope(m, pos_dram):
    S_ = m.S
    save = m.aoff
    areset(m)
    posi = aalloc(m, [T], I32)
    tt = aalloc(m, [T], F32)
    ti = aalloc(m, [T], I32)
    tf = aalloc(m, [T], F32)
    m.aoff = save
    invf = m.invf
    S_.dma("sp", posi, bcast_rows(pos_dram[0:1, :], 128))
    S_.dve(lambda e: e.tensor_copy(out=tt, in_=posi), [posi], [tt])
    S_.dve(lambda e: e.tensor_scalar(out=tt, in0=tt, scalar1=invf[:, 0:1], scalar2=None, op0=ALU.mult), [tt, invf[:]], [tt])
    for dst, shift in ((m.sinT, 0.0), (m.cosT, 0.25)):
        if shift:
            S_.dve(lambda e: e.tensor_scalar(out=tt, in0=tt, scalar1=shift, scalar2=None, op0=ALU.add), [tt], [tt])
        S_.dve(lambda e: e.tensor_copy(out=ti, in_=tt), [tt], [ti])
        S_.dve(lambda e: e.tensor_copy(out=tf, in_=ti), [ti], [tf])
        S_.dve(lambda e: e.tensor_tensor(out=tf, in0=tt, in1=tf, op=ALU.subtract), [tt, tf], [tf])
        S_.act(lambda e, dst=dst: e.activation(out=dst, in_=tf, func=AF.Sin, scale=6.2831845), [tf], [dst])


def rope_evac(m, p, M, sl, dst, scale=1.0):
    S_ = m.S
    xb = m.xb[m.xi % 2]
    m.xi += 1
    t1 = next_tmp(m)
    t2 = next_tmp(m)
    S_.act(lambda e: e.copy(out=xb[0:M, :], in_=p[0:M, :]), [p[0:M, :]], [xb[0:M, :]])
    p2 = next_psum(m)
    S_.pe(lambda e: e.matmul(p2[0:M, :], m.rotB[0:M, 0:M], xb[0:M, :], start=True, stop=True), [m.rotB[0:M, 0:M], xb[0:M, :]], [p2[0:M, :]])
    S_.dve(lambda e: e.tensor_tensor(out=t1[0:M, :], in0=m.cosT[0:M, sl], in1=p[0:M, :], op=ALU.mult), [p[0:M, :], m.cosT[0:M, sl]], [t1[0:M, :]])
    S_.dve(lambda e: e.tensor_tensor(out=t2[0:M, :], in0=m.sinT[0:M, sl], in1=p2[0:M, :], op=ALU.mult), [p2[0:M, :], m.sinT[0:M, sl]], [t2[0:M, :]])
    if scale == 1.0:
        S_.dve(lambda e: e.tensor_tensor(out=dst, in0=t1[0:M, :], in1=t2[0:M, :], op=ALU.add), [t1[0:M, :], t2[0:M, :]], [dst])
    else:
        S_.dve(lambda e: e.tensor_tensor(out=t1[0:M, :], in0=t1[0:M, :], in1=t2[0:M, :], op=ALU.add), [t1[0:M, :], t2[0:M, :]], [t1[0:M, :]])
        S_.act(lambda e: e.activation(out=dst, in_=t1[0:M, :], func=AF.Copy, scale=float(scale)), [t1[0:M, :]], [dst])


def fm_chunk(m, wv, c_lo, M, xT, nk, dst_dram, scale=None, rope=False, sb_dst=None):
    S_ = m.S
    stg = None
    if sb_dst is None:
        stg = m.stg[m.si % len(m.stg)]
    m.si += 1
    for half in range(2):
        sl = slice(half * 512, (half + 1) * 512)
        p = next_psum(m)
        mm_fm(m, p, wv, c_lo, M, xT, nk, sl)
        d_ = (sb_dst if sb_dst is not None else stg)[0:M, sl]
        if rope:
            rope_evac(m, p, M, sl, d_, scale if scale else 1.0)
        elif scale is not None:
            S_.act(lambda e, d_=d_, p=p: e.activation(out=d_, in_=p[0:M, :], func=AF.Copy, scale=float(scale)), [p[0:M, :]], [d_])
        elif half == 0:
            S_.act(lambda e, d_=d_, p=p: e.copy(out=d_, in_=p[0:M, :]), [p[0:M, :]], [d_])
        else:
            S_.dve(lambda e, d_=d_, p=p: e.tensor_copy(out=d_, in_=p[0:M, :]), [p[0:M, :]], [d_])
    if sb_dst is None:
        S_.dma("sp", dst_dram, stg[0:M, :])


def tm_tile(m, rhs_fn, n, xT, nk, j, dst_dram, f32=False):
    S_ = m.S
    p = next_psum(m)
    for k in range(nk):
        lhs = xT[:, k, j * 128:(j + 1) * 128]
        rhs = rhs_fn(k)
        S_.pe(lambda e, lhs=lhs, rhs=rhs, k=k: e.matmul(p[:, 0:n], lhs, rhs, start=(k == 0), stop=(k == nk - 1)), [lhs, rhs], [p[:, 0:n]])
    if f32:
        st = m.stgf[m.svi % len(m.stgf)]
    else:
        st = m.stgv[m.svi % len(m.stgv)]
    m.svi += 1
    if m.svi % 2:
        S_.act(lambda e: e.copy(out=st[:, 0:n], in_=p[:, 0:n]), [p[:, 0:n]], [st[:, 0:n]])
    else:
        S_.dve(lambda e: e.tensor_copy(out=st[:, 0:n], in_=p[:, 0:n]), [p[:, 0:n]], [st[:, 0:n]])
    S_.dma("sp", dst_dram, st[:, 0:n])


def s1_A(m, w_in, d):
    xT = m.hnT
    for gq in range(4):
        wv = load_w(m, w_in, 0, KC, gq * 512, 512)
        for ci in range(4):
            fm_chunk(m, wv, ci * 128, 128, xT, KC, d["q"][gq * 4 + ci], scale=128 ** -0.5)
    wv = load_w(m, w_in, 0, KC, 2048, 512)
    for ci in range(4):
        fm_chunk(m, wv, ci * 128, 128, xT, KC, d["k"][ci])
    wv = load_w(m, w_in, 0, KC, 2560, 512)
    for j in range(8):
        tm_tile(m, lambda k, wv=wv: wv[:, k, 0:512], 512, xT, KC, j, d["v"][j * 128:(j + 1) * 128, :])
    for gq in range(4):
        wv = load_w(m, w_in, 0, KC, 3072 + gq * 512, 512)
        for ci in range(4):
            fm_chunk(m, wv, ci * 128, 128, xT, KC, d["qi"][gq * 4 + ci], rope=True)
    wv = load_w(m, w_in, 0, KC, 5120, 144)
    fm_chunk(m, wv, 0, 128, xT, KC, d["ki"][0], rope=True)
    for j in range(8):
        tm_tile(m, lambda k, wv=wv: wv[:, k, 128:144], 16, xT, KC, j, d["wi"][j * 128:(j + 1) * 128, :], f32=True)


def s1_B(m, w_in, d):
    xT = m.hnT
    for gq in range(4):
        wv = load_w(m, w_in, 0, KC, gq * 512, 512)
        for ci in range(4):
            fm_chunk(m, wv, ci * 128, 128, xT, KC, d["q"][gq * 4 + ci], scale=64 ** -0.5)
    for gq in range(4):
        wv = load_w(m, w_in, 0, KC, 2048 + gq * 512, 512)
        for ci in range(4):
            fm_chunk(m, wv, ci * 128, 128, xT, KC, d["k"][gq * 4 + ci])
    for gq in range(4):
        wv = load_w(m, w_in, 0, KC, 4096 + gq * 512, 512)
        for j in range(8):
            tm_tile(m, lambda k, wv=wv: wv[:, k, 0:512], 512, xT, KC, j, d["v"][j * 128:(j + 1) * 128, gq * 512:(gq + 1) * 512])


def s1_C(m, w_down, gq_sb, gkv_sb, w_uq, w_ukv, d):
    S_ = m.S
    xT = m.hnT
    wv = load_w(m, w_down, 0, KC, 1024, 64)
    fm_chunk(m, wv, 0, 64, xT, KC, d["kr"][0], rope=True)
    sc = 192 ** -0.5
    for which in range(2):
        wv = load_w(m, w_down, 0, KC, which * 512, 512)
        for ci in range(4):
            fm_chunk(m, wv, ci * 128, 128, xT, KC, None, sb_dst=m.cT[:, ci, :])
        rmsnorm_to(m, gq_sb if which == 0 else gkv_sb, m.cnT, src=m.cT, nkc=4, ones=m.ones512)
        if which == 0:
            for hg in range(2):
                wv = load_w(m, w_uq, 0, 4, hg * 1536, 1536)
                for hh in range(8):
                    h = hg * 8 + hh
                    fm_chunk(m, wv, hh * 192, 128, m.cnT, 4, d["qn"][h], scale=sc)
                    fm_chunk(m, wv, hh * 192 + 128, 64, m.cnT, 4, d["qr"][h], scale=sc, rope=True)
        else:
            for hg in range(2):
                wv = load_w(m, w_ukv, 0, 4, hg * 2048, 2048)
                for hh in range(8):
                    h = hg * 8 + hh
                    fm_chunk(m, wv, hh * 256, 128, m.cnT, 4, d["kn"][h])
                wv4 = wv.rearrange("p k (h c) -> p k h c", c=256)
                for a in range(2):
                    for j in range(8):
                        tm_tile(m, lambda k, a=a: wv4[:, k, 4 * a:4 * a + 4, 128:256], 512, m.cnT, 4, j,
                                d["v"][j * 128:(j + 1) * 128, (hg * 8 + 4 * a) * 128:(hg * 8 + 4 * a + 4) * 128])


def setup_attn_psum(m):
    m.psum_rot = m.psum[0:4]
    m.psum_o = m.psum[4:6]
    m.oi = 0


def next_psum_a(m):
    p = m.psum_rot[m.psi % len(m.psum_rot)]
    m.psi += 1
    return p


def attn_qtile(m, ntl, qk_fn, add_fn, bias_fn, v_fn, dv, oacc, pbufs):
    S_ = m.S
    ngrp = ntl // 4
    pending = None
    for g in range(ngrp + 1):
        cur = None
        if g < ngrp:
            p = next_psum_a(m)
            for tt in range(4):
                t = g * 4 + tt
                ops = list(qk_fn(t)) + [(m.identB[:], a) for a in add_fn(t)]
                pc = p[:, tt * 128:(tt + 1) * 128]
                for i, (lhs, rhs) in enumerate(ops):
                    S_.pe(lambda e, pc=pc, lhs=lhs, rhs=rhs, i=i, n=len(ops): e.matmul(pc, lhs, rhs, start=(i == 0), stop=(i == n - 1)),
                          [lhs, rhs], [pc])
            pb = pbufs[m.pbi % len(pbufs)]
            m.pbi += 1
            b = bias_fn(g)
            if b is None:
                S_.act(lambda e, pb=pb, p=p: e.activation(out=pb, in_=p[:], func=AF.Exp), [p[:]], [pb])
            else:
                S_.act(lambda e, pb=pb, p=p, b=b: e.activation(out=pb, in_=p[:], func=AF.Exp, bias=b), [p[:], b], [pb])
            cur = (g, pb)
        if pending is not None:
            g0, pb0 = pending
            for tt in range(4):
                t = g0 * 4 + tt
                lhs = pb0[:, tt * 128:(tt + 1) * 128]
                rhs = v_fn(t)
                S_.pe(lambda e, lhs=lhs, rhs=rhs, t=t: e.matmul(oacc[:, 0:dv + 1], lhs, rhs, start=(t == 0), stop=(t == ntl - 1)),
                      [lhs, rhs], [oacc[:, 0:dv + 1]])
        pending = cur


def attn_finish(m, oacc, dv, o_sb, col0):
    S_ = m.S
    rs = m.rsb[m.oi % 2]
    m.oi += 1
    S_.dve(lambda e: e.reciprocal(out=rs, in_=oacc[:, dv:dv + 1]), [oacc[:, dv:dv + 1]], [rs])
    d_ = o_sb[:, col0:col0 + dv]
    S_.dve(lambda e: e.tensor_scalar(out=d_, in0=oacc[:, 0:dv], scalar1=rs, scalar2=None, op0=ALU.mult), [oacc[:, 0:dv], rs], [d_])


def o_transpose(m, o_sb, chunk, j):
    S_ = m.S
    pt = m.psumT[m.pti % 2][:, 0:128]
    m.pti += 1
    S_.pe(lambda e: e.transpose(pt, o_sb, m.identB[:]), [o_sb, m.identB[:]], [pt])
    d_ = m.hnT[:, chunk, j * 128:(j + 1) * 128]
    S_.dve(lambda e: e.tensor_copy(out=d_, in_=pt), [pt], [d_])


def attn_common_bufs(m):
    m.pbufs = [aalloc(m, [512], BF16) for _ in range(3)]
    m.pbi = 0
    m.rsb = [aalloc(m, [1], F32) for _ in range(2)]
    m.osb = [aalloc(m, [128], BF16) for _ in range(2)]
    m.pti = 0


def ktiles(ap, t0, n):
    return ap[:, t0 * 128:(t0 + n) * 128]


def load_kT(m, dst, src, parts=128, nsplit=4):
    w = S // nsplit
    for i in range(nsplit):
        m.S.dma("sp", dst[0:parts, i * w:(i + 1) * w], src[0:parts, i * w:(i + 1) * w])


def load_V(m, dst, src, c0, ncols, nsplit=4):
    w = 64 // nsplit
    for i in range(nsplit):
        s_ = src[i * w * 128:(i + 1) * w * 128, c0:c0 + ncols].rearrange("(t p) c -> p t c", p=128)
        m.S.dma("sp", dst[:, i * w:(i + 1) * w, 0:ncols], s_)


def attn_C(m, d, cmask_d):
    S_ = m.S
    areset(m)
    knb = [aalloc(m, [S], BF16) for _ in range(2)]
    vb = [aalloc(m, [64, 129], BF16) for _ in range(2)]
    krT = aalloc(m, [S], BF16)
    cm = aalloc(m, [8, 128], BF16)
    qnb = [aalloc(m, [T], BF16) for _ in range(2)]
    qrb = [aalloc(m, [T], BF16) for _ in range(2)]
    attn_common_bufs(m)
    load_kT(m, krT, d["KR"][0], parts=64)
    S_.dma("sp", cm, cmask_d)
    for b in vb:
        S_.pool(lambda e, b=b: e.memset(b[:, :, 128:129], 1.0), [], [b[:, :, 128:129]])
    for h in range(16):
        kn = knb[h % 2]
        vv = vb[h % 2]
        qn = qnb[h % 2]
        qr = qrb[h % 2]
        load_kT(m, kn, d["KN"][h])
        load_V(m, vv, d["V"], h * 128, 128)
        S_.dma("sp", qn, d["qn"][h])
        S_.dma("sp", qr[0:64, :], d["qr"][h])
        for j in range(8):
            ntl = 8 * (j + 1)
            oacc = m.psum_o[(h * 8 + j) % 2]
            qs = slice(j * 128, (j + 1) * 128)

            def qk_fn(t, kn=kn, qn=qn, qr=qr, qs=qs):
                return [(kn[:, t * 128:(t + 1) * 128], qn[:, qs]), (krT[0:64, t * 128:(t + 1) * 128], qr[0:64, qs])]

            def add_fn(t, ntl=ntl):
                return [cm[:, t - (ntl - 8), :]] if t >= ntl - 8 else []

            attn_qtile(m, ntl, qk_fn, add_fn, lambda g: None, lambda t, vv=vv: vv[:, t, :], 128, oacc, m.pbufs)
            osb = m.osb[(h * 8 + j) % 2]
            attn_finish(m, oacc, 128, osb, 0)
            o_transpose(m, osb, h, j)


def hankel(G_dram, row, rowlen=2176, n=2048):
    return bass.AP(G_dram.tensor, row * rowlen, [[1, 128], [1, n]])


def bias_prologue(m, tab_dram, nrows, nheads, sel_dram, G_dram):
    S_ = m.S
    areset(m)
    nk = (nrows + 127) // 128
    tb = aalloc(m, [nk, nheads], BF16)
    sel = aalloc(m, [nk, 2176], BF16)
    gsb = aalloc(m, [2176], BF16)
    for k in range(nk):
        r0, r1 = k * 128, min(nrows, (k + 1) * 128)
        S_.dma("pool", tb[0:r1 - r0, k, :], tab_dram[r0:r1, :])
        S_.dma("pool", sel[0:r1 - r0, k, :], sel_dram[r0:r1, :])
    for b in range(5):
        c0 = b * 512
        n = min(512, 2176 - c0)
        p = next_psum(m)
        for k in range(nk):
            kp = min(nrows, (k + 1) * 128) - k * 128
            lhs = tb[0:kp, k, :]
            rhs = sel[0:kp, k, c0:c0 + n]
            S_.pe(lambda e, p=p, lhs=lhs, rhs=rhs, k=k, n=n: e.matmul(p[0:nheads, 0:n], lhs, rhs, start=(k == 0), stop=(k == nk - 1)),
                  [lhs, rhs], [p[0:nheads, 0:n]])
        S_.act(lambda e, p=p, c0=c0, n=n: e.copy(out=gsb[0:nheads, c0:c0 + n], in_=p[0:nheads, 0:n]), [p[0:nheads, 0:n]], [gsb[0:nheads, c0:c0 + n]])
    S_.dma("sp", G_dram[:, :], gsb[0:nheads, :])


NIT = 18


def idx_A(m, d, cmq_d, mt_dram):
    S_ = m.S
    areset(m)
    kiT = aalloc(m, [S], BF16)
    sc = aalloc(m, [S], F32)
    junk = aalloc(m, [S], BF16)
    qib = [aalloc(m, [16, 128], BF16) for _ in range(2)]
    dg = aalloc(m, [16, 128], BF16)
    Rb = [aalloc(m, [512], BF16) for _ in range(3)]
    mst = [aalloc(m, [4, 128], BF16) for _ in range(2)]
    wib = [aalloc(m, [16], F32) for _ in range(2)]
    cmq = aalloc(m, [8 * 128], F32)
    mxb = aalloc(m, [16], F32)
    mnb = aalloc(m, [16], F32)
    sm = aalloc(m, [8], F32)
    cnt = aalloc(m, [NIT], F32)
    load_kT(m, kiT, d["KI"][0])
    S_.dma("sp", cmq, cmq_d)
    ri = 0
    for j in range(8):
        ntl = 8 * (j + 1)
        nblk = ntl // 4
        qi = qib[j % 2]
        wi = wib[j % 2]
        S_.dma("sp", qi, d["qi"][:, :, j * 128:(j + 1) * 128].rearrange("h p t -> p h t"))
        S_.dma("sp", wi, d["wi"][j * 128:(j + 1) * 128, :])
        for h in range(16):
            S_.pool(lambda e, h=h, wi=wi: e.tensor_scalar(out=dg[:, h, :], in0=m.identB[:], scalar1=wi[:, h:h + 1], scalar2=None, op0=ALU.mult),
                    [m.identB[:], wi[:, h:h + 1]], [dg[:, h, :]])
        for blk in range(nblk):
            sacc = m.psum_o[blk % 2]
            ksl = slice(blk * 512, (blk + 1) * 512)
            prev = None
            for h in range(17):
                cur = None
                if h < 16:
                    p = next_psum_a(m)
                    lhs = qi[:, h, :]
                    S_.pe(lambda e, p=p, lhs=lhs, ksl=ksl: e.matmul(p[:], lhs, kiT[:, ksl], start=True, stop=True), [lhs, kiT[:, ksl]], [p[:]])
                    R = Rb[ri % 3]
                    ri += 1
                    if h % 2 == 0:
                        S_.act(lambda e, R=R, p=p: e.activation(out=R, in_=p[:], func=AF.Relu), [p[:]], [R])
                    else:
                        S_.dve(lambda e, R=R, p=p: e.tensor_scalar(out=R, in0=p[:], scalar1=0.0, scalar2=None, op0=ALU.max), [p[:]], [R])
                    cur = (h, R)
                if prev is not None:
                    h0, R0 = prev
                    S_.pe(lambda e, sacc=sacc, h0=h0, R0=R0: e.matmul(sacc[:], dg[:, h0, :], R0, start=(h0 == 0), stop=(h0 == 15)),
                          [dg[:, h0, :], R0], [sacc[:]])
                prev = cur
            S_.dve(lambda e, sacc=sacc, blk=blk: e.tensor_reduce(out=mnb[:, blk:blk + 1], in_=sacc[:], axis=AX.X, op=ALU.min), [sacc[:]], [mnb[:, blk:blk + 1]])
            if blk >= nblk - 2:
                cs = cmq[:, (blk - (nblk - 2)) * 512:(blk - (nblk - 2) + 1) * 512]
                S_.dve(lambda e, sacc=sacc, ksl=ksl, cs=cs: e.tensor_tensor(out=sc[:, ksl], in0=sacc[:], in1=cs, op=ALU.add), [sacc[:], cs], [sc[:, ksl]])
            else:
                S_.act(lambda e, sacc=sacc, ksl=ksl: e.copy(out=sc[:, ksl], in_=sacc[:]), [sacc[:]], [sc[:, ksl]])
            S_.dve(lambda e, ksl=ksl, blk=blk: e.tensor_reduce(out=mxb[:, blk:blk + 1], in_=sc[:, ksl], axis=AX.X, op=ALU.max), [sc[:, ksl]], [mxb[:, blk:blk + 1]])
        nk_ = ntl * 128
        mx, t, step, cand, ge = (sm[:, i:i + 1] for i in range(5))
        S_.dve(lambda e: e.tensor_reduce(out=mx, in_=mxb[:, 0:nblk], axis=AX.X, op=ALU.max), [mxb[:, 0:nblk]], [mx])
        S_.dve(lambda e: e.tensor_reduce(out=t, in_=mnb[:, 0:nblk], axis=AX.X, op=ALU.min), [mnb[:, 0:nblk]], [t])
        S_.dve(lambda e: e.tensor_tensor(out=step, in0=mx, in1=t, op=ALU.subtract), [mx, t], [step])
        S_.dve(lambda e: e.memset(cnt, 0.0), [], [cnt])
        for it in range(NIT):
            c_ = cnt[:, it:it + 1]
            S_.dve(lambda e: e.tensor_scalar(out=step, in0=step, scalar1=0.5, scalar2=None, op0=ALU.mult), [step], [step])
            S_.dve(lambda e: e.tensor_tensor(out=cand, in0=t, in1=step, op=ALU.add), [t, step], [cand])
            S_.dve(lambda e, c_=c_, nk_=nk_: e.tensor_scalar(out=junk[:, 0:nk_], in0=sc[:, 0:nk_], scalar1=cand, scalar2=0.0, op0=ALU.is_ge, op1=ALU.add, accum_out=c_),
                   [sc[:, 0:nk_], cand, c_], [junk[:, 0:nk_], c_])
            S_.dve(lambda e, c_=c_: e.tensor_scalar(out=ge, in0=c_, scalar1=256.0, scalar2=None, op0=ALU.is_ge), [c_], [ge])
            S_.dve(lambda e: e.scalar_tensor_tensor(out=t, in0=step, scalar=ge, in1=t, op0=ALU.mult, op1=ALU.add), [step, ge, t], [t])
        S_.dve(lambda e, nk_=nk_: e.tensor_scalar(out=junk[:, 0:nk_], in0=sc[:, 0:nk_], scalar1=t, scalar2=-30000.0, op0=ALU.is_lt, op1=ALU.mult),
               [sc[:, 0:nk_], t], [junk[:, 0:nk_]])
        for g4 in range(ntl // 4):
            pt = m.psumT[g4 % 2][:, 0:512]
            for tt in range(4):
                src = junk[:, (4 * g4 + tt) * 128:(4 * g4 + tt + 1) * 128]
                S_.pe(lambda e, pt=pt, tt=tt, src=src: e.transpose(pt[:, tt * 128:(tt + 1) * 128], src, m.identB[:]), [src, m.identB[:]], [pt[:, tt * 128:(tt + 1) * 128]])
            ms = mst[g4 % 2]
            pv = pt.rearrange("p (a b) -> p a b", a=4)
            if g4 % 2:
                S_.act(lambda e, ms=ms, pv=pv: e.copy(out=ms, in_=pv), [pt], [ms])
            else:
                S_.dve(lambda e, ms=ms, pv=pv: e.tensor_copy(out=ms, in_=pv), [pt], [ms])
            S_.dma("sp", mt_dram[j, :, 4 * g4:4 * g4 + 4, :], ms)


def attn_A(m, d, mt_dram, G_dram, farb):
    S_ = m.S
    areset(m)
    kb = aalloc(m, [S], BF16)
    vb = aalloc(m, [64, 129], BF16)
    mtb = [aalloc(m, [64, 128], BF16) for _ in range(2)]
    btb = aalloc(m, [4, 2048], BF16)
    qv = [aalloc(m, [4, 128], BF16) for _ in range(2)]
    attn_common_bufs(m)
    S_.pool(lambda e: e.memset(vb[:, :, 128:129], 1.0), [], [vb[:, :, 128:129]])
    for g in range(4):
        load_kT(m, kb, d["K"][g])
        load_V(m, vb, d["V"], g * 128, 128)
        for hh in range(4):
            S_.dma("sp", btb[:, hh, :], hankel(G_dram, 4 * g + hh))
        for j in range(8):
            ntl = 8 * (j + 1)
            mt = mtb[(g * 8 + j) % 2]
            q_ = qv[(g * 8 + j) % 2]
            S_.dma("sp", mt[:, 0:ntl, :], mt_dram[j, :, 0:ntl, :])
            S_.dma("sp", q_, d["q"][4 * g:4 * g + 4, :, j * 128:(j + 1) * 128].rearrange("h p t -> p h t"))
            for hh in range(4):
                h = 4 * g + hh
                oacc = m.psum_o[(j * 4 + hh) % 2]

                def qk_fn(t, q_=q_, hh=hh):
                    return [(kb[:, t * 128:(t + 1) * 128], q_[:, hh, :])]

                def add_fn(t, mt=mt, ntl=ntl, hh=hh):
                    lst = [mt[:, t, :]]
                    idx = t - ntl + 16
                    if idx >= 0:
                        ip = 15 - idx
                        lst.append(btb[:, hh, ip * 128:(ip + 1) * 128])
                    return lst

                def bias_fn(g4, ntl=ntl, h=h):
                    return farb[:, h:h + 1] if 4 * g4 < ntl - 16 else None

                attn_qtile(m, ntl, qk_fn, add_fn, bias_fn, lambda t: vb[:, t, :], 128, oacc, m.pbufs)
                osb = m.osb[(j * 4 + hh) % 2]
                attn_finish(m, oacc, 128, osb, 0)
                o_transpose(m, osb, h, j)


def attn_B(m, d, maskB_d, GB_dram):
    S_ = m.S
    areset(m)
    kb = [aalloc(m, [S], BF16) for _ in range(2)]
    vb = [aalloc(m, [64, 130], BF16) for _ in range(2)]
    btb = [aalloc(m, [2, 2048], BF16) for _ in range(2)]
    mb = aalloc(m, [16, 128], BF16)
    qb = [aalloc(m, [T], BF16) for _ in range(2)]
    attn_common_bufs(m)
    S_.dma("sp", mb, maskB_d)
    for b in vb:
        for e_ in range(2):
            S_.pool(lambda e, b=b, e_=e_: e.memset(b[:, :, e_ * 65 + 64:e_ * 65 + 65], 1.0), [], [b[:, :, e_ * 65 + 64:e_ * 65 + 65]])
    for hc in range(16):
        k_ = kb[hc % 2]
        v_ = vb[hc % 2]
        q_ = qb[hc % 2]
        bt = btb[hc % 2]
        load_kT(m, k_, d["K"][hc])
        for e_ in range(2):
            w = 16
            for i in range(4):
                s_ = d["V"][i * w * 128:(i + 1) * w * 128, (2 * hc + e_) * 64:(2 * hc + e_ + 1) * 64].rearrange("(t p) c -> p t c", p=128)
                S_.dma("sp", v_[:, i * w:(i + 1) * w, e_ * 65:e_ * 65 + 64], s_)
            S_.dma("sp", bt[:, e_, :], hankel(GB_dram, 2 * hc + e_))
        S_.dma("sp", q_, d["q"][hc])
        for j in range(8):
            t0 = max(0, 8 * j - 8)
            ntl = 8 * j + 8 - t0
            osb = m.osb[(hc * 8 + j) % 2]
            qs = slice(j * 128, (j + 1) * 128)
            for e_ in range(2):
                oacc = m.psum_o[e_]
                ps_ = slice(e_ * 64, (e_ + 1) * 64)

                def qk_fn(tl, k_=k_, q_=q_, ps_=ps_, t0=t0, qs=qs):
                    t = t0 + tl
                    return [(k_[ps_, t * 128:(t + 1) * 128], q_[ps_, qs])]

                def add_fn(tl, ntl=ntl, bt=bt, e_=e_):
                    ip = 15 - (tl + 16 - ntl)
                    return [mb[:, ip, :], bt[:, e_, ip * 128:(ip + 1) * 128]]

                attn_qtile(m, ntl, qk_fn, add_fn, lambda g: None, lambda tl, v_=v_, e_=e_, t0=t0: v_[:, t0 + tl, e_ * 65:(e_ + 1) * 65], 64, oacc, m.pbufs)
                attn_finish(m, oacc, 64, osb, e_ * 64)
            o_transpose(m, osb, hc, j)

LAYER_KIND = [0, 1, 2, 0]
A_IN = 5264


def loc_specs(kind):
    if kind == 0:
        return {"q": ([16, 128, T], BF16), "k": ([4, 128, T], BF16), "v": ([T, 512], BF16),
                "qi": ([16, 128, T], BF16), "ki": ([1, 128, T], BF16), "wi": ([T, 16], F32)}
    if kind == 1:
        return {"q": ([16, 128, T], BF16), "k": ([16, 128, T], BF16), "v": ([T, 2048], BF16)}
    return {"qn": ([16, 128, T], BF16), "qr": ([16, 64, T], BF16), "kn": ([16, 128, T], BF16),
            "kr": ([1, 64, T], BF16), "v": ([T, 2048], BF16)}


def glob_specs(kind):
    if kind == 0:
        return {"K": ([4, 128, S], BF16), "V": ([S, 512], BF16), "KI": ([1, 128, S], BF16)}
    if kind == 1:
        return {"K": ([16, 128, S], BF16), "V": ([S, 2048], BF16)}
    return {"KN": ([16, 128, S], BF16), "KR": ([1, 64, S], BF16), "V": ([S, 2048], BF16)}


OWN_KEYS = {0: ("q", "qi", "wi"), 1: ("q",), 2: ("qn", "qr")}
EXCH = {0: (("k", "K"), ("v", "V"), ("ki", "KI")), 1: (("k", "K"), ("v", "V")), 2: (("kn", "KN"), ("kr", "KR"), ("v", "V"))}


def build_prog(k):
    m = M()
    S_ = m.S
    setup_common(m)
    setup_arena(m)
    setup_attn_psum(m)
    ng = 4 * 16 + 8
    gains_d = m.inp("gains", [128, ng])
    gs = m.sb("gs", [128, ng], F32)
    S_.dma("sp", gs[:], gains_d[:, :])
    if k <= 3:
        pos_d = m.inp("pos", [1, T], I32)
        invf_d = m.inp("c_invf", [128, 1])
        rot_d = m.inp("c_rot", [128, 128])
        setup_consts2(m, invf_d, rot_d)
    if k == 0:
        x_d = m.inp("x", [T, D])
        areset(m)
        stage = [aalloc(m, [D], F32) for _ in range(2)]
        load_x(m, x_d, stage)
    else:
        h_in = m.inp("h_in", [128, KC * T])
        load_h(m, h_in)
        kind = LAYER_KIND[k - 1]
        d = {}
        for key in OWN_KEYS[kind]:
            shp, dt = loc_specs(kind)[key]
            d[key] = m.inp("own_" + key, shp, dt)
        for key, (shp, dt) in glob_specs(kind).items():
            d[key] = m.inp("g_" + key, shp, dt)
        w_o = m.inp("w_o", [D, D])
        if kind == 0:
            cmq_d = m.inp("pc_cmq", [128, 1024])
            sel_d = m.inp("pc_selA", [32, 2176])
            t5_d = m.inp("t5", [32, 16])
            mt_dram = m.scratch("mt_dram", [8, 128, 64, 128], BF16)
            G_dram = m.scratch("G_dram", [16, 2176], BF16)
            farb = m.sb("farb", [128, 16], F32)
            S_.dma("sp", farb[:], bcast_rows(t5_d[15:16, :], 128))
            bias_prologue(m, t5_d, 32, 16, sel_d, G_dram)
            idx_A(m, d, cmq_d, mt_dram)
            attn_A(m, d, mt_dram, G_dram, farb)
        elif kind == 1:
            mb_d = m.inp("pc_maskB", [128, 16, 128], BF16)
            sel_d = m.inp("pc_selB", [257, 2176])
            tab_d = m.inp("rel_table", [257, 32])
            G_dram = m.scratch("G_dram", [32, 2176], BF16)
            bias_prologue(m, tab_d, 257, 32, sel_d, G_dram)
            attn_B(m, d, mb_d, G_dram)
        else:
            cm_d = m.inp("pc_cmS", [128, 8, 128], BF16)
            attn_C(m, d, cm_d)
        w_fi = m.inp("ffn_w_in", [D, 2 * FF])
        w_fo = m.inp("ffn_w_out", [FF, D])
        w_pg = m.inp("ple_w_gate", [D, D])
        w_pp = m.inp("ple_w_proj", [256, D])
        p_d = m.inp("p", [T, 256])
        setup_linear_arena(m)
        out_proj(m, w_o, m.hnT)
        rmsnorm_to(m, gs[:, 0:16], m.hnT)
        ffn(m, w_fi, w_fo, m.actT)
        load_pT(m, p_d, m.pT, m.pstage)
        rmsnorm_to(m, gs[:, 16:32], m.hnT)
        ple(m, w_pg, w_pp, m.pT)
    outs = {}
    if k <= 3:
        kind = LAYER_KIND[k]
        if k == 0:
            setup_linear_arena(m)
        rmsnorm_to(m, gs[:, 32:48], m.hnT)
        setup_rope(m, pos_d)
        dl = {}
        for key, (shp, dt) in loc_specs(kind).items():
            dl[key] = m.outp("loc_" + key, shp, dt)
        if kind == 0:
            w_in = m.inp("w_in", [D, A_IN])
            s1_A(m, w_in, dl)
        elif kind == 1:
            w_in = m.inp("w_in", [D, 6144])
            s1_B(m, w_in, dl)
        else:
            w_dn = m.inp("w_down", [D, 1088])
            w_uq = m.inp("w_uq", [512, 3072])
            w_ukv = m.inp("w_ukv", [512, 4096])
            s1_C(m, w_dn, gs[:, 64:68], gs[:, 68:72], w_uq, w_ukv, dl)
        h_out = m.outp("h_out", [128, KC * T])
        save_h(m, h_out)
        fl = [h_out[:, :]] + [v[tuple(slice(None) for _ in v.shape)] for v in dl.values()]
        for f in fl:
            S_.fence("sp", [f])
    else:
        y_d = m.outp("y", [T, D])
        rms_stats(m, m.rstd, m.sqb)
        areset(m, 3 * WSLOT + 2048 + 2048)
        yb = [aalloc(m, [128], F32) for _ in range(2)]
        ob = [aalloc(m, [D], F32) for _ in range(2)]
        final_norm_out(m, gs[:, 32:48], m.rstd, y_d, yb, ob)
        S_.fence("sp", [y_d[:, :]])
    st = m.finish()
    return m, st


def arrange_gain(g):
    g = np.asarray(g, np.float32)
    return np.ascontiguousarray(g.reshape(-1, 128).T)


def core_rows(a, c):
    return np.ascontiguousarray(a.reshape((8, 8, 128) + a.shape[1:])[:, c].reshape((T,) + a.shape[1:]))


def per_core_consts(c):
    q = np.arange(128)
    sp = np.arange(128)
    s_in = 127 - sp
    out = {}
    u = np.arange(8)
    valid = (u[None, :, None] < c) | ((u[None, :, None] == c) & ((s_in[None, None, :] // 64) <= (q[:, None, None] // 64)))
    out["pc_cmq"] = np.where(valid, 0.0, -1e30).astype(np.float32).reshape(128, 1024)
    out["pc_cmS"] = np.where(valid.transpose(2, 1, 0), 0.0, -30000.0).astype(ml_dtypes.bfloat16)
    ip = np.arange(16)
    uu = 7 - ip
    diff = 2 * (c - uu)[None, :, None] + (q[None, None, :] // 64) - (s_in[:, None, None] // 64)
    out["pc_maskB"] = np.where((diff >= 0) & (diff <= 8), 0.0, -30000.0).astype(ml_dtypes.bfloat16)
    y = np.arange(2176)
    Cc = (7 - c) * 128 + 127
    rel = Cc - y
    n = np.abs(rel)
    thr = np.array([1, 2, 3, 4, 5, 6, 7, 8, 15, 27, 50, 91, 166, 305, 559])
    bucket = (n[:, None] >= thr[None, :]).sum(1) + np.where(rel > 0, 16, 0)
    selA = np.zeros((32, 2176), np.float32)
    selA[bucket, y] = 1.0
    out["pc_selA"] = selA
    r = np.clip(y - Cc, -128, 128) + 128
    selB = np.zeros((257, 2176), np.float32)
    selB[r, y] = 1.0
    out["pc_selB"] = selB
    return out


def gather_k(locs, rev=True):
    nch, P, _ = locs[0].shape
    st = np.stack([l.reshape(nch, P, 8, 128) for l in locs], axis=3)
    if rev:
        st = st[..., ::-1]
    return np.ascontiguousarray(st.reshape(nch, P, S))


def gather_v(locs, rev=True):
    C = locs[0].shape[1]
    st = np.stack([l.reshape(8, 128, C) for l in locs], axis=1)
    if rev:
        st = st[:, :, ::-1]
    return np.ascontiguousarray(st.reshape(S, C))


_PROGS = {}


def get_prog(k):
    if k not in _PROGS:
        _PROGS[k] = build_prog(k)[0]
    return _PROGS[k]


class Runner:
    def __init__(self, x, p, positions, t5_table, a_w_in, a_w_out, b_w_in, b_rel_table, b_w_out,
                 c_w_down, c_q_norm, c_kv_norm, c_w_uq, c_w_ukv, c_w_out,
                 attn_norm, ffn_norm, ffn_w_in, ffn_w_out, ple_norm, ple_w_gate, ple_w_proj, final_norm):
        f32 = lambda a: np.ascontiguousarray(np.asarray(a), dtype=np.float32)
        self.f32 = f32
        self.x = f32(x)[0]
        self.p = f32(p)[:, 0]
        self.pos = np.asarray(positions).astype(np.int32)[0]
        self.I = dict(t5_table=t5_table, b_rel_table=b_rel_table, c_w_down=c_w_down, c_q_norm=c_q_norm, c_kv_norm=c_kv_norm,
                      c_w_uq=c_w_uq, c_w_ukv=c_w_ukv, attn_norm=attn_norm, ffn_norm=ffn_norm, ffn_w_in=ffn_w_in,
                      ffn_w_out=ffn_w_out, ple_norm=ple_norm, ple_w_gate=ple_w_gate, ple_w_proj=ple_w_proj, final_norm=final_norm)
        self.ident = np.eye(128, dtype=np.float32)
        invf = np.zeros((128, 1), np.float32)
        invf[:64, 0] = (10000.0 ** (-(np.arange(64) % 32) * 2.0 / 64)) / (2 * np.pi)
        self.invf = invf
        rot = np.zeros((128, 128), np.float32)
        for i in range(32):
            rot[i + 32, i] = -1.0
            rot[i, i + 32] = 1.0
        self.rot = rot
        self.pcc = [per_core_consts(c) for c in range(NCORE)]
        self.w_mix_in = [a_w_in[0], b_w_in[0], None, a_w_in[1]]
        self.w_mix_out = [a_w_out[0], b_w_out[0], c_w_out[0], a_w_out[1]]
        self.h = [None] * NCORE
        self.loc = None
        self.y = None

    def step(self, k):
        f32 = self.f32
        I = self.I
        prog = get_prog(k)
        common = {"c_identF": self.ident}
        g = np.zeros((128, 72), np.float32)
        if k >= 1:
            g[:, 0:16] = arrange_gain(I["ffn_norm"][k - 1])
            g[:, 16:32] = arrange_gain(I["ple_norm"][k - 1])
        g[:, 32:48] = arrange_gain(I["attn_norm"][k] if k <= 3 else I["final_norm"])
        if k == 2:
            g[:, 64:68] = arrange_gain(I["c_q_norm"][0])
            g[:, 68:72] = arrange_gain(I["c_kv_norm"][0])
        common["gains"] = g
        if k <= 3:
            common["c_invf"] = self.invf
            common["c_rot"] = self.rot
            kind = LAYER_KIND[k]
            if kind in (0, 1):
                common["w_in"] = f32(self.w_mix_in[k])
            else:
                common["w_down"] = f32(I["c_w_down"][0])
                common["w_uq"] = f32(I["c_w_uq"][0])
                common["w_ukv"] = f32(I["c_w_ukv"][0])
        loc = self.loc
        if k >= 1:
            i = k - 1
            kindp = LAYER_KIND[i]
            common["w_o"] = f32(self.w_mix_out[i])
            common["ffn_w_in"] = f32(I["ffn_w_in"][i])
            common["ffn_w_out"] = f32(I["ffn_w_out"][i])
            common["ple_w_gate"] = f32(I["ple_w_gate"][i])
            common["ple_w_proj"] = f32(I["ple_w_proj"][i])
            if kindp == 0:
                common["t5"] = f32(I["t5_table"])
            elif kindp == 1:
                common["rel_table"] = f32(I["b_rel_table"][0])
            for lk, gk in EXCH[kindp]:
                if lk.startswith("v"):
                    common["g_" + gk] = gather_v([np.asarray(loc[c]["loc_" + lk]) for c in range(NCORE)])
                else:
                    common["g_" + gk] = gather_k([np.asarray(loc[c]["loc_" + lk]) for c in range(NCORE)])
        in_maps = []
        for c in range(NCORE):
            im = dict(common)
            if k == 0:
                im["x"] = core_rows(self.x, c)
            else:
                im["h_in"] = self.h[c]
                im["p"] = core_rows(self.p[k - 1], c)
                kindp = LAYER_KIND[k - 1]
                for key in OWN_KEYS[kindp]:
                    im["own_" + key] = loc[c]["loc_" + key]
                if kindp == 0:
                    im["pc_cmq"] = self.pcc[c]["pc_cmq"]
                    im["pc_selA"] = self.pcc[c]["pc_selA"]
                elif kindp == 1:
                    im["pc_maskB"] = self.pcc[c]["pc_maskB"]
                    im["pc_selB"] = self.pcc[c]["pc_selB"]
                else:
                    im["pc_cmS"] = self.pcc[c]["pc_cmS"]
            if k <= 3:
                im["pos"] = core_rows(self.pos, c)[None, :]
            in_maps.append(im)
        res = run_bass_kernel_spmd(prog.nc, in_maps, core_ids=list(range(NCORE)))
        if k <= 3:
            self.loc = res.results
            self.h = [res.results[c]["h_out"] for c in range(NCORE)]
        else:
            y = np.zeros((8, 8, 128, D), np.float32)
            for c in range(NCORE):
                y[:, c] = np.asarray(res.results[c]["y"], np.float32).reshape(8, 128, D)
            self.y = y.reshape(1, S, D)
        return res


def kernel(**inputs):
    r = Runner(**inputs)
    for k in range(5):
        r.step(k)
    return r.y
```
